# Optimizing a Trainium2 kernel written in Bass

```python
import jax, jax.numpy as jnp
from jax import lax
import numpy as np

D_MODEL = 2048
BATCH = 2
SEQ = 8192
DEPTH = 1

CHUNK = 64
N_META = 16
Q_BLOCK = 128
SB_HEADS = 8
SB_HEAD_DIM = 128
SB_WIDTH = SB_HEADS * SB_HEAD_DIM
SB_SCALE = SB_HEAD_DIM ** -0.5
MLA_HEADS = 16
MLA_NOPE_DIM = 128
MLA_ROPE_DIM = 64
MLA_V_DIM = 128
MLA_KV_RANK = 512
MLA_QK_DIM = MLA_NOPE_DIM + MLA_ROPE_DIM
MLA_SCALE = MLA_QK_DIM ** -0.5
ROPE_THETA = 10000.0
IN_SPLITS = (SB_WIDTH, SB_WIDTH, SB_WIDTH, MLA_HEADS * MLA_QK_DIM, MLA_KV_RANK, MLA_ROPE_DIM, D_MODEL, D_MODEL)
IN_WIDTH = SB_WIDTH * 3 + MLA_HEADS * MLA_QK_DIM + MLA_KV_RANK + MLA_ROPE_DIM + 2 * D_MODEL
N_GROUPS = 4
EXPERTS_PER_GROUP = 8
N_EXPERTS = N_GROUPS * EXPERTS_PER_GROUP
TOP_K = 2
EXPERT_FF = 1024
ROUTE_BLOCK = 256
RMS_EPS = 1e-6

kernel_name = "hybrid_stickbreak_mla_hmoe_block"


def rms_norm(x, g):
    xf = x.astype(jnp.float32)
    y = xf * lax.rsqrt(jnp.mean(xf * xf, axis=-1, keepdims=True) + RMS_EPS)
    return (y * g.astype(jnp.float32)).astype(x.dtype)


def rope(x, pos):
    half = MLA_ROPE_DIM // 2
    inv = ROPE_THETA ** (-jnp.arange(half, dtype=jnp.float32) / half)
    ang = pos.astype(jnp.float32)[:, None] * inv[None, :]
    cos = jnp.cos(ang).astype(x.dtype)
    sin = jnp.sin(ang).astype(x.dtype)
    x1, x2 = x[..., :half], x[..., half:]
    return jnp.concatenate([x1 * cos - x2 * sin, x1 * sin + x2 * cos], axis=-1)


def to_blocks(t):
    b, h, lp, d = t.shape
    return t.reshape(b, h, lp // Q_BLOCK, Q_BLOCK, d).transpose(2, 0, 1, 3, 4)


def from_blocks(t):
    nb, b, h, qb, d = t.shape
    return t.transpose(1, 2, 0, 3, 4).reshape(b, h, nb * qb, d)


def stick_breaking_attention(q, k, v, idx):
    nb = q.shape[2] // Q_BLOCK
    def one(args):
        qb, qpos = args
        z = jnp.einsum('bhqd,bhkd->bhqk', qb, k).astype(jnp.float32) * SB_SCALE
        causal = idx[None, :] < qpos[:, None]
        log_1m = jnp.where(causal, jax.nn.log_sigmoid(-z), 0.0)
        log_w = jax.nn.log_sigmoid(z) + lax.cumsum(log_1m, axis=3, reverse=True) - log_1m
        a = jnp.where(causal, jnp.exp(log_w), 0.0)
        return jnp.einsum('bhqk,bhkd->bhqd', a.astype(v.dtype), v)
    o = lax.map(one, (to_blocks(q), idx.reshape(nb, Q_BLOCK)))
    return from_blocks(o)


def latent_attention(q_nope, q_rope, k_nope, k_rope, v, chunk):
    nb = q_nope.shape[2] // Q_BLOCK
    def one(args):
        qn, qr, qc = args
        s = (jnp.einsum('bhqd,bhkd->bhqk', qn, k_nope)
             + jnp.einsum('bhqr,bkr->bhqk', qr, k_rope)).astype(jnp.float32) * MLA_SCALE
        mask = chunk[None, :] <= qc[:, None]
        p = jax.nn.softmax(jnp.where(mask, s, -jnp.inf), axis=-1)
        return jnp.einsum('bhqk,bhkd->bhqd', p.astype(v.dtype), v)
    o = lax.map(one, (to_blocks(q_nope), to_blocks(q_rope), chunk.reshape(nb, Q_BLOCK)))
    return from_blocks(o)


def hybrid_mixer(hn, w_in, b_gate, kv_norm_g, w_uk, w_uv, w_proj_a, w_proj_b, w_out):
    b, l, _ = hn.shape
    lp = -(-l // Q_BLOCK) * Q_BLOCK
    hp = jnp.pad(hn, ((0, 0), (0, lp - l), (0, 0)))
    idx = jnp.arange(lp)
    chunk = jnp.where(idx < N_META, 0, 1 + (idx - N_META) // CHUNK)
    offs = [sum(IN_SPLITS[:i + 1]) for i in range(len(IN_SPLITS) - 1)]
    sb_q, sb_k, sb_v, mla_q, c_kv, k_rope, g_a, g_b = jnp.split(hp @ w_in, offs, axis=-1)

    def heads(t, h):
        return t.reshape(b, lp, h, -1).transpose(0, 2, 1, 3)

    o_a = stick_breaking_attention(heads(sb_q, SB_HEADS), heads(sb_k, SB_HEADS), heads(sb_v, SB_HEADS), idx)

    q = heads(mla_q, MLA_HEADS)
    q_nope, q_rope = q[..., :MLA_NOPE_DIM], rope(q[..., MLA_NOPE_DIM:], idx)
    k_rope = rope(k_rope, idx)
    c = rms_norm(c_kv, kv_norm_g)
    k_nope = heads(c @ w_uk, MLA_HEADS)
    v = heads(c @ w_uv, MLA_HEADS)
    o_b = latent_attention(q_nope, q_rope, k_nope, k_rope, v, chunk)

    def flat(o):
        return o.transpose(0, 2, 1, 3).reshape(b, lp, -1)[:, :l]

    bg_a, bg_b = jnp.split(b_gate, 2)
    y = (jax.nn.sigmoid(g_a[:, :l] + bg_a) * (flat(o_a) @ w_proj_a)
         + jax.nn.sigmoid(g_b[:, :l] + bg_b) * (flat(o_b) @ w_proj_b))
    return y @ w_out


def hierarchical_moe(hn, w_route_group, b_route_group, w_route_expert, b_route_expert, w1, w3, w2):
    n, d = hn.shape
    g_logits = (hn @ w_route_group).astype(jnp.float32) + b_route_group.astype(jnp.float32)
    g_prob = jax.nn.softmax(g_logits, axis=-1)
    g_sel = jnp.argmax(g_logits, axis=-1)
    p_g = jnp.take_along_axis(g_prob, g_sel[:, None], axis=-1)
    e_logits = ((hn @ w_route_expert).astype(jnp.float32)
                + b_route_expert.astype(jnp.float32)).reshape(n, N_GROUPS, EXPERTS_PER_GROUP)
    e_in_group = jnp.take_along_axis(e_logits, g_sel[:, None, None], axis=1)[:, 0]
    top_v, top_i = lax.top_k(jax.nn.softmax(e_in_group, axis=-1), TOP_K)
    weights = p_g * top_v / jnp.sum(top_v, axis=-1, keepdims=True)
    expert_id = g_sel[:, None] * EXPERTS_PER_GROUP + top_i

    m = n * TOP_K
    flat_e = expert_id.reshape(m)
    flat_w = weights.reshape(m).astype(hn.dtype)
    flat_t = jnp.arange(m) // TOP_K
    order = jnp.argsort(flat_e)
    se, st, sw = flat_e[order], flat_t[order], flat_w[order]
    counts = jnp.bincount(flat_e, length=N_EXPERTS)
    padded = (counts + ROUTE_BLOCK - 1) // ROUTE_BLOCK * ROUTE_BLOCK
    starts = jnp.cumsum(counts) - counts
    pends = jnp.cumsum(padded)
    pstarts = pends - padded
    dest = pstarts[se] + jnp.arange(m) - starts[se]
    n_blocks = (m + N_EXPERTS * (ROUTE_BLOCK - 1) + ROUTE_BLOCK - 1) // ROUTE_BLOCK
    p_rows = n_blocks * ROUTE_BLOCK
    buf_t = jnp.zeros((p_rows,), jnp.int32).at[dest].set(st.astype(jnp.int32))
    buf_w = jnp.zeros((p_rows,), hn.dtype).at[dest].set(sw)
    block_e = jnp.clip(jnp.searchsorted(pends, jnp.arange(n_blocks) * ROUTE_BLOCK, side='right'), 0, N_EXPERTS - 1)

    def expert_block(args):
        tok, w, e = args
        xb = hn[tok]
        hid = jax.nn.silu(xb @ w1[e]) * (xb @ w3[e])
        return (hid @ w2[e]) * w[:, None]
    y = lax.map(expert_block, (buf_t.reshape(n_blocks, ROUTE_BLOCK), buf_w.reshape(n_blocks, ROUTE_BLOCK), block_e))
    return jnp.zeros_like(hn).at[buf_t].add(y.reshape(p_rows, d))


def setup_inputs(seed: int = 0) -> dict:
    key = jax.random.key(seed)
    ks = jax.random.split(key, 20)
    f32 = jnp.float32

    def nrm(k, shape, fan_in):
        return jax.random.normal(k, shape, f32) * (fan_in ** -0.5)

    def gain(k, shape):
        return 1.0 + 0.02 * jax.random.normal(k, shape, f32)

    return {
        "x": jax.random.normal(ks[0], (BATCH, SEQ, D_MODEL), f32),
        "meta_tokens": jax.random.normal(ks[1], (N_META, D_MODEL), f32),
        "norm1_g": gain(ks[2], (DEPTH, D_MODEL)),
        "w_in": nrm(ks[3], (DEPTH, D_MODEL, IN_WIDTH), D_MODEL),
        "b_gate": 0.02 * jax.random.normal(ks[4], (DEPTH, 2 * D_MODEL), f32),
        "kv_norm_g": gain(ks[5], (DEPTH, MLA_KV_RANK)),
        "w_uk": nrm(ks[6], (DEPTH, MLA_KV_RANK, MLA_HEADS * MLA_NOPE_DIM), MLA_KV_RANK),
        "w_uv": nrm(ks[7], (DEPTH, MLA_KV_RANK, MLA_HEADS * MLA_V_DIM), MLA_KV_RANK),
        "w_proj_a": nrm(ks[8], (DEPTH, SB_WIDTH, D_MODEL), SB_WIDTH),
        "w_proj_b": nrm(ks[9], (DEPTH, MLA_HEADS * MLA_V_DIM, D_MODEL), MLA_HEADS * MLA_V_DIM),
        "w_out": nrm(ks[10], (DEPTH, D_MODEL, D_MODEL), D_MODEL),
        "norm2_g": gain(ks[11], (DEPTH, D_MODEL)),
        "w_route_group": nrm(ks[12], (DEPTH, D_MODEL, N_GROUPS), D_MODEL),
        "b_route_group": 0.01 * jax.random.normal(ks[13], (DEPTH, N_GROUPS), f32),
        "w_route_expert": nrm(ks[14], (DEPTH, D_MODEL, N_EXPERTS), D_MODEL),
        "b_route_expert": 0.01 * jax.random.normal(ks[15], (DEPTH, N_EXPERTS), f32),
        "w1": nrm(ks[16], (DEPTH, N_EXPERTS, D_MODEL, EXPERT_FF), D_MODEL),
        "w3": nrm(ks[17], (DEPTH, N_EXPERTS, D_MODEL, EXPERT_FF), D_MODEL),
        "w2": nrm(ks[18], (DEPTH, N_EXPERTS, EXPERT_FF, D_MODEL), EXPERT_FF),
        "final_g": gain(ks[19], (D_MODEL,)),
    }


def reference(x, meta_tokens, norm1_g, w_in, b_gate, kv_norm_g, w_uk, w_uv, w_proj_a, w_proj_b, w_out,
              norm2_g, w_route_group, b_route_group, w_route_expert, b_route_expert, w1, w3, w2, final_g):
    b = x.shape[0]
    meta = jnp.broadcast_to(meta_tokens.astype(x.dtype)[None], (b, N_META, x.shape[-1]))
    h = jnp.concatenate([meta, x], axis=1)
    for l in range(DEPTH):
        h = h + hybrid_mixer(rms_norm(h, norm1_g[l]), w_in[l], b_gate[l], kv_norm_g[l], w_uk[l], w_uv[l],
                             w_proj_a[l], w_proj_b[l], w_out[l])
        hn = rms_norm(h, norm2_g[l])
        ffn = hierarchical_moe(hn.reshape(-1, hn.shape[-1]), w_route_group[l], b_route_group[l],
                               w_route_expert[l], b_route_expert[l], w1[l], w3[l], w2[l])
        h = h + ffn.reshape(h.shape)
    return rms_norm(h, final_g)[:, N_META:]
```

```python
import contextlib
import numpy as np
import concourse.bass as bass
import concourse.mybir as mybir
from concourse.bass_utils import run_bass_kernel_spmd

F32 = mybir.dt.float32
BF16 = mybir.dt.bfloat16
I32 = mybir.dt.int32
AF = mybir.ActivationFunctionType
ALU = mybir.AluOpType
AX = mybir.AxisListType

D = 2048
NTOK = 8320
NTP = 8704
NOWN = 2176
NREAL = 2080
IN_WIDTH = 10816
SB_SCALE = 128 ** -0.5
MLA_SCALE = 192 ** -0.5
EPS = 1e-6
CAP = 256
NSLOT = 32 * CAP


class T:
    __slots__ = ("w", "r")

    def __init__(self):
        self.w = None
        self.r = {}


class Eng:
    def __init__(self, B, eng, name):
        self.B, self.e, self.name = B, eng, name
        self.sem = None
        self.count = 0
        self.waited = {}
        self.pend_r, self.pend_w = [], []

    def wait(self, tok):
        if tok is None:
            return
        sem, val = tok
        k = id(sem)
        if self.waited.get(k, 0) >= val:
            return
        self.e.wait_ge(sem, val)
        self.waited[k] = val

    def signal(self, inst):
        if self.sem is None or self.count >= 30000:
            self.sem = self.B.new_sem(self.name)
            self.count = 0
        self.count += 1
        inst.then_inc(self.sem, 1)
        return (self.sem, self.count)


class Builder:
    NDMA = 40

    def __init__(self, nc, stack):
        self.nc, self.stack = nc, stack
        self.nsem = 0
        self.pe = Eng(self, nc.tensor, "pe")
        self.act = Eng(self, nc.scalar, "act")
        self.dve = Eng(self, nc.vector, "dve")
        self.pool = Eng(self, nc.gpsimd, "pool")
        self.sp = Eng(self, nc.sync, "sp")
        self.engs = [self.pe, self.act, self.dve, self.pool, self.sp]
        self.dsems, self.dcount = [], []
        self.dnext = 0
        self.alt = 0

    def new_sem(self, name):
        self.nsem += 1
        return self.stack.enter_context(self.nc.semaphore(f"{name}_{self.nsem}"))

    def _deps(self, E, reads, writes):
        for t in reads:
            E.wait(t.w)
        for t in writes:
            E.wait(t.w)
            for tok in t.r.values():
                E.wait(tok)

    def _commit(self, tok, reads, writes):
        k = id(tok[0])
        for t in reads:
            t.r[k] = tok
        for t in writes:
            t.w = tok
            t.r = {}

    def op(self, E, fn, reads=(), writes=(), sig=True):
        self._deps(E, reads, writes)
        inst = fn()
        if not sig:
            E.pend_r.extend(reads)
            E.pend_w.extend(writes)
            return None
        tok = E.signal(inst)
        self._commit(tok, list(reads) + E.pend_r, list(writes) + E.pend_w)
        E.pend_r, E.pend_w = [], []
        return tok

    def _dsem(self, E):
        if len(self.dsems) < self.NDMA:
            self.dsems.append(self.new_sem("dma"))
            self.dcount.append(0)
            i = len(self.dsems) - 1
        else:
            i = self.dnext
            self.dnext = (self.dnext + 1) % self.NDMA
            E.wait((self.dsems[i], self.dcount[i]))
        self.dcount[i] += 16
        return i

    def dma(self, E, out, in_, reads=(), writes=(), **kw):
        self._deps(E, reads, writes)
        i = self._dsem(E)
        E.e.dma_start(out=out, in_=in_, **kw).then_inc(self.dsems[i], 16)
        tok = (self.dsems[i], self.dcount[i])
        self._commit(tok, reads, writes)
        return tok

    def idma(self, out, out_off, in_, in_off, reads=(), writes=(), **kw):
        E = self.pool
        self._deps(E, reads, writes)
        i = self._dsem(E)
        E.e.indirect_dma_start(out, out_off, in_, in_off, **kw).then_inc(self.dsems[i], 16)
        tok = (self.dsems[i], self.dcount[i])
        self._commit(tok, reads, writes)
        return tok

    def barrier(self):
        toks = [(E.sem, E.count) for E in self.engs if E.sem is not None]
        toks += [(s, c) for s, c in zip(self.dsems, self.dcount) if c > 0]
        for E in self.engs:
            for tok in toks:
                if tok[0] is not E.sem:
                    E.wait(tok)


class Ring:
    def __init__(self, items):
        self.items = items
        self.i = 0

    def next(self):
        it = self.items[self.i]
        self.i = (self.i + 1) % len(self.items)
        return it


def build_nc(upto=99, debug=False):
    nc = bass.Bass("TRN2", target_bir_lowering=False)

    def din(name, shape, dt=F32):
        return nc.dram_tensor(name, list(shape), dt, kind="ExternalInput").ap()

    def dscr(name, shape, dt):
        return nc.dram_tensor(name, list(shape), dt, kind="ExternalOutput" if debug else "Internal").ap()

    hall = din("hall", [NTP, D])
    hown = din("hown", [NOWN, D])
    ropek = din("ropek", [NTP, 64])
    ropeq = din("ropeq", [NOWN, 64])
    sbmask_d = din("sbmask", [128, 512])
    mlamask_d = din("mlamask", [128, 640])
    norm1_g = din("norm1_g", [1, D])
    w_in = din("w_in", [D, IN_WIDTH])
    b_gate = din("b_gate", [1, 4096])
    kv_norm_g = din("kv_norm_g", [1, 512])
    w_uk = din("w_uk", [512, 2048])
    w_uv = din("w_uv", [512, 2048])
    w_proj_a = din("w_proj_a", [1024, D])
    w_proj_b = din("w_proj_b", [2048, D])
    w_out = din("w_out", [D, D])
    norm2_g = din("norm2_g", [1, D])
    w_rg = din("w_route_group", [D, 4])
    b_rg = din("b_route_group", [1, 4])
    w_re = din("w_route_expert", [D, 32])
    b_re = din("b_route_expert", [1, 32])
    w1 = din("w1", [32, D, 1024])
    w3 = din("w3", [32, D, 1024])
    w2 = din("w2", [32, 1024, D])
    final_g = din("final_g", [1, D])
    out = nc.dram_tensor("out", [NOWN, D], F32, kind="ExternalOutput").ap()

    KT_sb = dscr("KT_sb", [8, 128, NTP], BF16)
    V_sb = dscr("V_sb", [NTP, 1024], BF16)
    KnT = dscr("KnT", [16, 128, NTP], BF16)
    KrT = dscr("KrT", [64, NTP], BF16)
    Vm = dscr("Vm", [NTP, 2048], BF16)
    QT_sb = dscr("QT_sb", [8, 128, NOWN], BF16)
    QnT = dscr("QnT", [16, 128, NOWN], BF16)
    QrT = dscr("QrT", [16, 64, NOWN], BF16)
    GT = dscr("GT", [32, 128, NOWN], BF16)
    OT = dscr("OT", [24, 128, NOWN], BF16)
    YT = dscr("YT", [16, 128, NOWN], BF16)
    H1 = dscr("H1", [NOWN, D], F32)
    Xs = dscr("Xs", [NSLOT, D], BF16)
    Ys = dscr("Ys", [2 * NSLOT, 1024], F32)

    def bc_rows(ap2d, n):
        return bass.AP(ap2d.tensor, ap2d.offset, [[0, 128], [1, n]])

    with contextlib.ExitStack() as st:
        B = Builder(nc, st)
        pe, act, dve, pool, sp = B.pe, B.act, B.dve, B.pool, B.sp

        uid = [0]

        def sbt(stack, name, shape, dt):
            uid[0] += 1
            return stack.enter_context(nc.sbuf_tensor(f"{name}_u{uid[0]}", list(shape), dt))

        pf = Ring([(st.enter_context(nc.psum_tensor(f"pf{i}", [128, 512], F32)), T()) for i in range(4)])
        pacc = Ring([(st.enter_context(nc.psum_tensor(f"pacc{i}", [128, 512], F32)), T()) for i in range(2)])
        pb = Ring([(st.enter_context(nc.psum_tensor(f"pb{i}", [128, 1024], BF16)), T()) for i in range(2)])

        ident = sbt(st, "ident", [128, 128], BF16)
        identf = sbt(st, "identf", [128, 128], F32)
        onesf = sbt(st, "onesf", [128, 512], F32)
        epst = sbt(st, "epst", [128, 1], F32)
        Tc = T()
        st.enter_context(nc.Block())
        B.op(pool, lambda: nc.gpsimd.memset(onesf[:], 1.0), writes=[Tc])
        B.op(pool, lambda: nc.gpsimd.memset(epst[:], EPS), writes=[Tc])
        B.op(pool, lambda: nc.gpsimd.memset(identf[:], 0.0), writes=[Tc])
        B.op(pool, lambda: nc.gpsimd.affine_select(identf[:], onesf[:, 0:128], [[1, 128]], ALU.is_equal, 0.0,
                                                   base=0, channel_multiplier=-1), reads=[Tc], writes=[Tc])
        B.op(dve, lambda: nc.vector.tensor_copy(ident[:], identf[:]), reads=[Tc], writes=[Tc])

        def evac(out_ap, in_ap, reads, writes):
            B.alt ^= 1
            if B.alt:
                return B.op(act, lambda: nc.scalar.copy(out_ap, in_ap), reads=reads, writes=writes)
            return B.op(dve, lambda: nc.vector.tensor_copy(out_ap, in_ap), reads=reads, writes=writes)

        def rstd_from(src_ap, n, junk, small, Tsrc, Tjunk, Tsmall):
            B.op(act, lambda: nc.scalar.activation(out=junk[:, 0:n], in_=src_ap, func=AF.Square,
                                                   accum_out=small[:, 0:1]),
                 reads=[Tsrc], writes=[Tjunk, Tsmall])
            B.op(act, lambda: nc.scalar.activation(out=small[:, 1:2], in_=small[:, 0:1], func=AF.Sqrt,
                                                   scale=1.0 / n, bias=epst[:, 0:1]),
                 reads=[Tsmall, Tc], writes=[Tsmall])
            B.op(dve, lambda: nc.vector.reciprocal(small[:, 2:3], small[:, 1:2]), reads=[Tsmall], writes=[Tsmall])

        def transposes(src, Tsrc, nblk, width, dst_fn, Tdst, rows=128):
            per = 8
            for i0 in range(0, nblk, per):
                n = min(per, nblk - i0)
                pbt, Tpb = pb.next()
                for i in range(n):
                    B.op(pe, lambda i=i: nc.tensor.transpose(pbt[0:width, i * 128:(i + 1) * 128],
                                                             src[:, (i0 + i) * width:(i0 + i + 1) * width], ident[:]),
                         reads=[Tsrc, Tc], writes=[Tpb], sig=(i == n - 1))
                evac(dst_fn(i0, n), pbt[0:width, 0:n * 128].rearrange("p (a b) -> p a b", b=128), [Tpb], [Tdst])

        for part in ((0, 1) if upto >= 1 else ()):
            with contextlib.ExitStack() as ph:
                g1 = sbt(ph, "g1", [128, D], F32)
                TW = T()
                w_in_r = w_in.rearrange("(c p) n -> p c n", p=128)
                if part == 0:
                    Wkv = sbt(ph, "Wkv", [128, 16, 2048], BF16)
                    for c0 in range(0, 16, 4):
                        B.dma(pool, Wkv[:, c0:c0 + 4, 0:2048], w_in_r[:, c0:c0 + 4, 1024:3072], writes=[TW])
                else:
                    Wkv = sbt(ph, "Wkvb", [128, 16, 576], BF16)
                    Wuk = sbt(ph, "Wuk", [128, 4, 2048], BF16)
                    Wuv = sbt(ph, "Wuv", [128, 4, 2048], BF16)
                    gkv = sbt(ph, "gkv", [128, 512], F32)
                    for c0 in range(0, 16, 4):
                        B.dma(pool, Wkv[:, c0:c0 + 4, :], w_in_r[:, c0:c0 + 4, 6144:6720], writes=[TW])
                    B.dma(pool, Wuk[:], w_uk.rearrange("(c p) n -> p c n", p=128), writes=[TW])
                    B.dma(pool, Wuv[:], w_uv.rearrange("(c p) n -> p c n", p=128), writes=[TW])
                    B.dma(sp, gkv[:], bc_rows(kv_norm_g, 512), writes=[TW])
                B.dma(sp, g1[:], bc_rows(norm1_g, D), writes=[TW])

                xr = Ring([(sbt(ph, f"xt{i}", [128, D], F32), T()) for i in range(2)])
                junk = sbt(ph, "junk", [128, D], BF16)
                Tjunk = T()
                smr = Ring([(sbt(ph, f"sm{i}", [128, 4], F32), T()) for i in range(4)])
                hnbr = Ring([(sbt(ph, f"hnb{i}", [128, D], BF16), T()) for i in range(2)])
                hnTr = Ring([(sbt(ph, f"hnT{i}", [128, 16, 512], BF16), T()) for i in range(2)])
                cTr = Ring([(sbt(ph, f"cT{i}", [128, 4, 512], BF16), T()) for i in range(2)])
                krTr = Ring([(sbt(ph, f"krT{i}", [128, 512], BF16), T()) for i in range(2)])
                stg = Ring([(sbt(ph, f"stg{i}", [128, 512], BF16), T()) for i in range(3)])
                vst = Ring([(sbt(ph, f"vst{i}", [128, 2048], BF16), T()) for i in range(2)])
                cnb = Ring([(sbt(ph, f"cnb{i}", [128, 512], BF16), T()) for i in range(2)])
                rkr = Ring([(sbt(ph, f"rk{i}", [128, 64], F32), T()) for i in range(2)])
                rtmp = Ring([(sbt(ph, f"rtmp{i}", [128, 128], F32), T()) for i in range(2)])
                krb = Ring([(sbt(ph, f"krb{i}", [128, 128], BF16), T()) for i in range(2)])
                for (kb0, Tkb0) in krb.items:
                    B.op(pool, lambda kb0=kb0: nc.gpsimd.memset(kb0[:, 64:128], 0.0), writes=[Tkb0])

                ngroups = 17
                gws = [512] * 16 + [128]

                def normE(g, t):
                    row0 = g * 512 + t * 128
                    xt, Txt = xr.next()
                    B.dma(sp, xt[:], hall[row0:row0 + 128, :], writes=[Txt])
                    sm, Tsm = smr.next()
                    rstd_from(xt[:], D, junk, sm, Txt, Tjunk, Tsm)
                    hnb, Thnb = hnbr.next()
                    B.op(dve, lambda: nc.vector.scalar_tensor_tensor(out=hnb[:], in0=xt[:], scalar=sm[:, 2:3],
                                                                     in1=g1[:], op0=ALU.mult, op1=ALU.mult),
                         reads=[Txt, Tsm, TW], writes=[Thnb])
                    return hnb, Thnb

                def normX(hnb, Thnb, hnT, ThnT, t):
                    transposes(hnb, Thnb, 16, 128,
                               lambda i0, n, t=t: hnT[:, i0:i0 + n, t * 128:(t + 1) * 128], ThnT)

                def proj_group(g, hnT, ThnT):
                    gw = gws[g]
                    nt = gw // 128
                    col0 = g * 512
                    for h in (range(8) if part == 0 else ()):
                        pz, Tpz = pf.next()
                        for c in range(16):
                            B.op(pe, lambda c=c: nc.tensor.matmul(pz[:, 0:gw], Wkv[:, c, h * 128:(h + 1) * 128],
                                                                  hnT[:, c, 0:gw], start=(c == 0), stop=(c == 15)),
                                 reads=[TW, ThnT], writes=[Tpz], sig=(c == 15))
                        sg, Tsg = stg.next()
                        evac(sg[:, 0:gw], pz[:, 0:gw], [Tpz], [Tsg])
                        B.dma(sp, KT_sb[h, :, col0:col0 + gw], sg[:, 0:gw], reads=[Tsg])
                        yield
                    for t in (range(nt) if part == 0 else ()):
                        row0 = col0 + t * 128
                        vs, Tvs = vst.next()
                        for bnk in range(2):
                            pz, Tpz = pf.next()
                            for c in range(16):
                                B.op(pe, lambda c=c: nc.tensor.matmul(
                                    pz[:, :], hnT[:, c, t * 128:(t + 1) * 128],
                                    Wkv[:, c, 1024 + bnk * 512:1024 + (bnk + 1) * 512],
                                    start=(c == 0), stop=(c == 15)), reads=[TW, ThnT], writes=[Tpz], sig=(c == 15))
                            evac(vs[:, bnk * 512:(bnk + 1) * 512], pz[:, :], [Tpz], [Tvs])
                            yield
                        B.dma(sp, V_sb[row0:row0 + 128, :], vs[:, 0:1024], reads=[Tvs])
                    if part == 0:
                        return
                    cT, TcT = cTr.next()
                    krT, TkrT = krTr.next()
                    deferred = []

                    def flush_deferred():
                        for (cn_, Tcn_, kb_, Tkb_, t_) in deferred:
                            transposes(cn_, Tcn_, 4, 128, lambda i0, n, t_=t_: cT[:, i0:i0 + n, t_ * 128:(t_ + 1) * 128], TcT)
                            transposes(kb_, Tkb_, 1, 128, lambda i0, n, t_=t_: krT[:, t_ * 128:(t_ + 1) * 128].rearrange("p (a b) -> p a b", b=128), TkrT)
                        deferred.clear()

                    for t in range(nt):
                        row0 = col0 + t * 128
                        pc, Tpc = pf.next()
                        for c in range(16):
                            B.op(pe, lambda c=c: nc.tensor.matmul(pc[:, :], hnT[:, c, t * 128:(t + 1) * 128],
                                                                  Wkv[:, c, 0:512], start=(c == 0), stop=(c == 15)),
                                 reads=[TW, ThnT], writes=[Tpc], sig=(c == 15))
                        pk, Tpk = pf.next()
                        for c in range(16):
                            B.op(pe, lambda c=c: nc.tensor.matmul(pk[:, 0:64], hnT[:, c, t * 128:(t + 1) * 128],
                                                                  Wkv[:, c, 512:576], start=(c == 0), stop=(c == 15)),
                                 reads=[TW, ThnT], writes=[Tpk], sig=(c == 15))
                        sm, Tsm = smr.next()
                        rstd_from(pc[:, :], 512, junk, sm, Tpc, Tjunk, Tsm)
                        cn, Tcn = cnb.next()
                        B.op(dve, lambda: nc.vector.scalar_tensor_tensor(out=cn[:], in0=pc[:, :], scalar=sm[:, 2:3],
                                                                         in1=gkv[:], op0=ALU.mult, op1=ALU.mult),
                             reads=[Tpc, Tsm, TW], writes=[Tcn])
                        rk, Trk = rkr.next()
                        B.dma(sp, rk[:], ropek[row0:row0 + 128, :], writes=[Trk])
                        tmp, Ttmp = rtmp.next()
                        kb, Tkb = krb.next()
                        x1, x2 = pk[:, 0:32], pk[:, 32:64]
                        cs, sn = rk[:, 0:32], rk[:, 32:64]
                        B.op(dve, lambda: nc.vector.tensor_tensor(tmp[:, 0:32], x1, cs, ALU.mult), reads=[Tpk, Trk], writes=[Ttmp])
                        B.op(dve, lambda: nc.vector.tensor_tensor(tmp[:, 32:64], x2, sn, ALU.mult), reads=[Tpk, Trk], writes=[Ttmp])
                        B.op(dve, lambda: nc.vector.tensor_tensor(tmp[:, 64:96], x1, sn, ALU.mult), reads=[Tpk, Trk], writes=[Ttmp])
                        B.op(dve, lambda: nc.vector.tensor_tensor(tmp[:, 96:128], x2, cs, ALU.mult), reads=[Tpk, Trk], writes=[Ttmp])
                        B.op(dve, lambda: nc.vector.tensor_tensor(kb[:, 0:32], tmp[:, 0:32], tmp[:, 32:64], ALU.subtract), reads=[Ttmp], writes=[Tkb])
                        B.op(dve, lambda: nc.vector.tensor_tensor(kb[:, 32:64], tmp[:, 64:96], tmp[:, 96:128], ALU.add), reads=[Ttmp], writes=[Tkb])
                        flush_deferred()
                        deferred.append((cn, Tcn, kb, Tkb, t))
                        yield
                        yield
                    flush_deferred()
                    B.dma(sp, KrT[:, col0:col0 + gw], krT[0:64, 0:gw], reads=[TkrT])
                    for h in range(16):
                        pz, Tpz = pf.next()
                        for rc in range(4):
                            B.op(pe, lambda rc=rc: nc.tensor.matmul(pz[:, 0:gw], Wuk[:, rc, h * 128:(h + 1) * 128],
                                                                    cT[:, rc, 0:gw], start=(rc == 0), stop=(rc == 3)),
                                 reads=[TW, TcT], writes=[Tpz], sig=(rc == 3))
                        sg, Tsg = stg.next()
                        evac(sg[:, 0:gw], pz[:, 0:gw], [Tpz], [Tsg])
                        B.dma(sp, KnT[h, :, col0:col0 + gw], sg[:, 0:gw], reads=[Tsg])
                        yield
                    for t in range(nt):
                        row0 = col0 + t * 128
                        vs, Tvs = vst.next()
                        for bnk in range(4):
                            pz, Tpz = pf.next()
                            for rc in range(4):
                                B.op(pe, lambda rc=rc: nc.tensor.matmul(pz[:, :], cT[:, rc, t * 128:(t + 1) * 128],
                                                                        Wuv[:, rc, bnk * 512:(bnk + 1) * 512],
                                                                        start=(rc == 0), stop=(rc == 3)),
                                     reads=[TW, TcT], writes=[Tpz], sig=(rc == 3))
                            evac(vs[:, bnk * 512:(bnk + 1) * 512], pz[:, :], [Tpz], [Tvs])
                            yield
                        B.dma(sp, Vm[row0:row0 + 128, :], vs[:, :], reads=[Tvs])

                hnT_cur = hnTr.next()
                for t in range(gws[0] // 128):
                    hb = normE(0, t)
                    normX(*hb, *hnT_cur, t)
                for g in range(ngroups):
                    nt = gws[g] // 128
                    n_items = (8 + 2 * nt) if part == 0 else (2 * nt + 16 + 4 * nt)
                    ntn = gws[g + 1] // 128 if g + 1 < ngroups else 0
                    hnT_nxt = hnTr.next() if ntn else None
                    marks = {max(1, -(-(t + 1) * n_items // ntn) - (1 if t == ntn - 1 else 0)): t for t in range(ntn)}
                    pend = normE(g + 1, 0) if ntn else None
                    k = 0
                    for _ in proj_group(g, *hnT_cur):
                        k += 1
                        if k in marks:
                            t = marks[k]
                            normX(*pend, *hnT_nxt, t)
                            pend = normE(g + 1, t + 1) if t + 1 < ntn else None
                    assert pend is None, (g, k, n_items, marks)
                    hnT_cur = hnT_nxt
                B.barrier()

        own_groups = [(0, 512), (512, 512), (1024, 512), (1536, 512), (2048, 128)]
        if upto >= 2:
            ph0 = contextlib.ExitStack()
            hnTo = sbt(ph0, "hnTo", [128, 16, NOWN], BF16)
            ThnTo = T()
            bgT = sbt(ph0, "bgT", [128, 32], F32)
            TW = T()
            B.dma(sp, bgT[:], b_gate.rearrange("o (c p) -> p (o c)", p=128), writes=[TW],
                  allow_slow_non_contiguous=True)
            with contextlib.ExitStack() as ph:
                g1 = sbt(ph, "g1b", [128, D], F32)
                B.dma(sp, g1[:], bc_rows(norm1_g, D), writes=[TW])
                xr = Ring([(sbt(ph, f"xt2{i}", [128, D], F32), T()) for i in range(2)])
                junk = sbt(ph, "junk2", [128, D], BF16)
                Tjunk = T()
                smr = Ring([(sbt(ph, f"sm2{i}", [128, 4], F32), T()) for i in range(4)])
                hnbr = Ring([(sbt(ph, f"hnb2{i}", [128, D], BF16), T()) for i in range(2)])
                for t in range(17):
                    xt, Txt = xr.next()
                    B.dma(sp, xt[:], hown[t * 128:(t + 1) * 128, :], writes=[Txt])
                    sm, Tsm = smr.next()
                    rstd_from(xt[:], D, junk, sm, Txt, Tjunk, Tsm)
                    hnb, Thnb = hnbr.next()
                    B.op(dve, lambda: nc.vector.scalar_tensor_tensor(out=hnb[:], in0=xt[:], scalar=sm[:, 2:3],
                                                                     in1=g1[:], op0=ALU.mult, op1=ALU.mult),
                         reads=[Txt, Tsm, TW], writes=[Thnb])
                    transposes(hnb, Thnb, 16, 128, lambda i0, n, t=t: hnTo[:, i0:i0 + n, t * 128:(t + 1) * 128], ThnTo)
                B.barrier()
            with ph0 as ph:
                Wcr = Ring([(sbt(ph, f"Wc{i}", [128, 16, 512], BF16), T()) for i in range(2)])
                stq = Ring([(sbt(ph, f"stq{i}", [128, NOWN], BF16), T()) for i in range(3)])
                w_in_r = w_in.rearrange("(c p) n -> p c n", p=128)

                def fm_chunk(load_fn, dst_fn, gate_f0=None):
                    Wc, TWc = Wcr.next()
                    load_fn(Wc, TWc)
                    for fb in range(4):
                        sq, Tsq = stq.next()
                        for (c0, gw) in own_groups:
                            pz, Tpz = pf.next()
                            for c in range(16):
                                B.op(pe, lambda c=c: nc.tensor.matmul(pz[:, 0:gw], Wc[:, c, fb * 128:(fb + 1) * 128],
                                                                      hnTo[:, c, c0:c0 + gw], start=(c == 0), stop=(c == 15)),
                                     reads=[TWc, ThnTo], writes=[Tpz], sig=(c == 15))
                            if gate_f0 is None:
                                evac(sq[:, c0:c0 + gw], pz[:, 0:gw], [Tpz], [Tsq])
                            else:
                                f = gate_f0 + fb
                                B.op(act, lambda: nc.scalar.activation(out=sq[:, c0:c0 + gw], in_=pz[:, 0:gw],
                                                                       func=AF.Sigmoid, bias=bgT[:, f:f + 1], scale=1.0),
                                     reads=[Tpz, TW], writes=[Tsq])
                        B.dma(sp, dst_fn(fb), sq[:, :], reads=[Tsq])

                for i in range(2):
                    fm_chunk(lambda Wc, TWc, i=i: B.dma(pool, Wc[:], w_in_r[:, :, i * 512:(i + 1) * 512], writes=[TWc]),
                             lambda fb, i=i: QT_sb[4 * i + fb])
                for i in range(4):
                    def ld(Wc, TWc, i=i):
                        for hh in range(4):
                            cc = 3072 + (4 * i + hh) * 192
                            B.dma(pool, Wc[:, :, hh * 128:(hh + 1) * 128], w_in_r[:, :, cc:cc + 128], writes=[TWc])
                    fm_chunk(ld, lambda fb, i=i: QnT[4 * i + fb])
                for i in range(8):
                    fm_chunk(lambda Wc, TWc, i=i: B.dma(pool, Wc[:], w_in_r[:, :, 6720 + i * 512:6720 + (i + 1) * 512], writes=[TWc]),
                             lambda fb, i=i: GT[4 * i + fb], gate_f0=4 * i)
                rqr = Ring([(sbt(ph, f"rq{i}", [128, 64], F32), T()) for i in range(2)])
                rtmp = Ring([(sbt(ph, f"rtq{i}", [128, 4, 8, 32], F32), T()) for i in range(2)])
                qrb = Ring([(sbt(ph, f"qrb{i}", [128, 8, 64], BF16), T()) for i in range(2)])
                qrst = sbt(ph, "qrst", [128, 4, NOWN], BF16)
                Tqrst = T()
                for i in range(2):
                    Wc, TWc = Wcr.next()
                    for hh in range(8):
                        cc = 3072 + (8 * i + hh) * 192 + 128
                        B.dma(pool, Wc[:, :, hh * 64:(hh + 1) * 64], w_in_r[:, :, cc:cc + 64], writes=[TWc])
                    for t in range(17):
                        pz, Tpz = pf.next()
                        for c in range(16):
                            B.op(pe, lambda c=c: nc.tensor.matmul(pz[:, :], hnTo[:, c, t * 128:(t + 1) * 128], Wc[:, c, :],
                                                                  start=(c == 0), stop=(c == 15)),
                                 reads=[TWc, ThnTo], writes=[Tpz], sig=(c == 15))
                        rq, Trq = rqr.next()
                        B.dma(sp, rq[:], ropeq[t * 128:(t + 1) * 128, :], writes=[Trq])
                        tmp, Ttmp = rtmp.next()
                        qb, Tqb = qrb.next()
                        pz3 = pz[:, :].rearrange("p (h e) -> p h e", e=64)
                        for hh in range(8):
                            x1, x2 = pz3[:, hh, 0:32], pz3[:, hh, 32:64]
                            cs, sn = rq[:, 0:32], rq[:, 32:64]
                            B.op(dve, lambda: nc.vector.tensor_tensor(tmp[:, 0, hh, :], x1, cs, ALU.mult), reads=[Tpz, Trq], writes=[Ttmp])
                            B.op(dve, lambda: nc.vector.tensor_tensor(tmp[:, 1, hh, :], x2, sn, ALU.mult), reads=[Tpz, Trq], writes=[Ttmp])
                            B.op(dve, lambda: nc.vector.tensor_tensor(tmp[:, 2, hh, :], x1, sn, ALU.mult), reads=[Tpz, Trq], writes=[Ttmp])
                            B.op(dve, lambda: nc.vector.tensor_tensor(tmp[:, 3, hh, :], x2, cs, ALU.mult), reads=[Tpz, Trq], writes=[Ttmp])
                        B.op(pool, lambda: nc.gpsimd.tensor_tensor(qb[:, :, 0:32], tmp[:, 0, :, :], tmp[:, 1, :, :], ALU.subtract), reads=[Ttmp], writes=[Tqb])
                        B.op(pool, lambda: nc.gpsimd.tensor_tensor(qb[:, :, 32:64], tmp[:, 2, :, :], tmp[:, 3, :, :], ALU.add), reads=[Ttmp], writes=[Tqb])
                        qb2 = qb[:, :, :].rearrange("p h e -> p (h e)")
                        transposes(qb2, Tqb, 4, 128, lambda i0, n, t=t: qrst[:, i0:i0 + n, t * 128:(t + 1) * 128], Tqrst)
                    for hh in range(8):
                        B.dma(sp, QrT[8 * i + hh], qrst[(hh % 2) * 64:(hh % 2) * 64 + 64, hh // 2, :], reads=[Tqrst])
                B.barrier()

        if upto >= 3:
            with contextlib.ExitStack() as ph:
                sbm = sbt(ph, "sbm", [128, 512], F32)
                TM = T()
                B.dma(sp, sbm[:], sbmask_d, writes=[TM])
                KTr = Ring([(sbt(ph, f"KT{i}", [128, NTOK], BF16), T()) for i in range(2)])
                Vr = Ring([(sbt(ph, f"Vh{i}", [128, 65, 128], BF16), T()) for i in range(2)])
                Qr = Ring([(sbt(ph, f"QT{i}", [128, NOWN], BF16), T()) for i in range(2)])
                zmr = Ring([(sbt(ph, f"zm{i}", [128, 512], F32), T()) for i in range(2)])
                btr = Ring([(sbt(ph, f"bt{i}", [128, 512], F32), T()) for i in range(3)])
                omr = Ring([(sbt(ph, f"om{i}", [128, 512], F32), T()) for i in range(3)])
                obr = Ring([(sbt(ph, f"ob{i}", [128, 516], F32), T()) for i in range(4)])
                Ar = Ring([(sbt(ph, f"A{i}", [128, 512], BF16), T()) for i in range(4)])
                ATr = Ring([(sbt(ph, f"AT{i}", [128, 512], BF16), T()) for i in range(3)])
                Ost = Ring([(sbt(ph, f"Ost{i}", [128, NOWN], BF16), T()) for i in range(2)])
                V_sb_r = V_sb.rearrange("(b p) n -> p b n", p=128)

                def load_head(h):
                    KTt, TKT = KTr.next()
                    B.dma(sp, KTt[:], KT_sb[h, :, 0:NTOK], writes=[TKT])
                    Vt, TV = Vr.next()
                    B.dma(sp, Vt[:], V_sb_r[:, 0:65, h * 128:(h + 1) * 128], writes=[TV])
                    Qt, TQ = Qr.next()
                    B.dma(sp, Qt[:], QT_sb[h], writes=[TQ])
                    return KTt, TKT, Vt, TV, Qt, TQ

                nxt = load_head(0)
                LAG = 2
                for h in range(8):
                    KTt, TKT, Vt, TV, Qt, TQ = nxt
                    if h + 1 < 8:
                        nxt = load_head(h + 1)
                    Os, TOs = Ost.next()
                    descs = []
                    for m in range(17):
                        for c in range(m, -1, -1):
                            descs.append((m, c))
                    st3 = {"prev_ob": None, "po": {}, "done": {}, "A": {}}

                    def stageA1(i):
                        m, c = descs[i]
                        kw = 512 if c < 16 else 128
                        pz, Tpz = pf.next()
                        B.op(pe, lambda: nc.tensor.matmul(pz[:, 0:kw], Qt[:, m * 128:(m + 1) * 128],
                                                          KTt[:, c * 512:c * 512 + kw], start=True, stop=True),
                             reads=[TQ, TKT], writes=[Tpz])
                        st3["z"][i] = (pz, Tpz)

                    def stageA2(i):
                        m, c = descs[i]
                        kw = 512 if c < 16 else 128
                        pz, Tpz = st3["z"].pop(i)
                        src, Tsrc = pz, Tpz
                        if c == m:
                            st3["prev_ob"] = None
                            zm, Tzm = zmr.next()
                            B.op(dve, lambda: nc.vector.tensor_tensor(zm[:, 0:kw], pz[:, 0:kw], sbm[:, 0:kw], ALU.add),
                                 reads=[Tpz, TM], writes=[Tzm])
                            src, Tsrc = zm, Tzm
                        om, Tom = omr.next()
                        B.op(act, lambda: nc.scalar.activation(out=om[:, 0:kw], in_=src[:, 0:kw], func=AF.Sigmoid, scale=-SB_SCALE),
                             reads=[Tsrc], writes=[Tom])
                        ob, Tob = obr.next()
                        if st3["prev_ob"] is None:
                            B.op(pool, lambda: nc.gpsimd.memset(ob[:, kw:kw + 1], 1.0), writes=[Tob])
                            init = 1.0
                            rd = [Tom, Tc]
                        else:
                            pob, Tpob = st3["prev_ob"]
                            B.op(pool, lambda: nc.gpsimd.tensor_copy(ob[:, kw:kw + 1], pob[:, 0:1]), reads=[Tpob], writes=[Tob])
                            init = pob[:, 0:1]
                            rd = [Tom, Tc, Tpob]
                        B.op(dve, lambda: nc.vector.tensor_tensor_scan(ob[:, 0:kw][:, ::-1], om[:, 0:kw][:, ::-1],
                                                                       onesf[:, 0:kw], init, ALU.mult, ALU.mult),
                             reads=rd, writes=[Tob])
                        A, TA = Ar.next()
                        if i % 3 == 2:
                            B.op(pool, lambda: nc.gpsimd.tensor_tensor(A[:, 0:kw], ob[:, 1:kw + 1], ob[:, 0:kw], ALU.subtract),
                                 reads=[Tob], writes=[TA])
                        else:
                            B.op(dve, lambda: nc.vector.tensor_tensor(A[:, 0:kw], ob[:, 1:kw + 1], ob[:, 0:kw], ALU.subtract),
                                 reads=[Tob], writes=[TA])
                        st3["prev_ob"] = (ob, Tob)
                        st3["A"][i] = (A, TA)

                    def stageB(i):
                        m, c = descs[i]
                        kw = 512 if c < 16 else 128
                        nbk = kw // 128
                        A, TA = st3["A"].pop(i)
                        if c == m:
                            st3["po"][m] = pacc.next()
                            st3["done"][m] = 0
                        po, Tpo = st3["po"][m]
                        nblk_total = sum((4 if cc < 16 else 1) for cc in range(m + 1))
                        AT, TAT = ATr.next()
                        pbt, Tpb = pb.next()
                        for ii in range(nbk):
                            B.op(pe, lambda: nc.tensor.transpose(pbt[:, ii * 128:(ii + 1) * 128], A[:, ii * 128:(ii + 1) * 128], ident[:]),
                                 reads=[TA, Tc], writes=[Tpb], sig=(ii == nbk - 1))
                        B.op(act, lambda: nc.scalar.copy(AT[:, 0:kw], pbt[:, 0:kw]), reads=[Tpb], writes=[TAT])
                        for ii in range(nbk):
                            done = st3["done"][m]
                            B.op(pe, lambda: nc.tensor.matmul(po[:, 0:128], Vt[:, c * 4 + ii, :], AT[:, ii * 128:(ii + 1) * 128],
                                                              start=(done == 0), stop=(done == nblk_total - 1)),
                                 reads=[TV, TAT], writes=[Tpo], sig=(ii == nbk - 1))
                            st3["done"][m] = done + 1
                        if c == 0:
                            evac(Os[:, m * 128:(m + 1) * 128], po[:, 0:128], [Tpo], [TOs])

                    nd = len(descs)
                    st3["z"] = {}
                    for s_ in range(nd + 3):
                        if s_ < nd:
                            stageA1(s_)
                        if 0 <= s_ - 1 < nd:
                            stageA2(s_ - 1)
                        if s_ - 3 >= 0:
                            stageB(s_ - 3)
                    B.dma(sp, OT[h], Os[:, :], reads=[TOs])
                B.barrier()

        if upto >= 4:
            with contextlib.ExitStack() as ph:
                mmf = sbt(ph, "mmf", [128, 640], F32)
                mmb = sbt(ph, "mmb", [128, 640], BF16)
                TM = T()
                B.dma(sp, mmf[:], mlamask_d, writes=[TM])
                B.op(dve, lambda: nc.vector.tensor_copy(mmb[:], mmf[:]), reads=[TM], writes=[TM])
                KrTt = sbt(ph, "KrTt", [128, NTOK], BF16)
                TKr = T()
                B.op(pool, lambda: nc.gpsimd.memset(KrTt[64:128, :], 0.0), writes=[TKr])
                B.dma(sp, KrTt[0:64, :], KrT[:, 0:NTOK], writes=[TKr])
                KTr = Ring([(sbt(ph, f"KnT{i}", [128, NTOK], BF16), T()) for i in range(2)])
                Vr = Ring([(sbt(ph, f"Vm{i}", [128, 65, 128], BF16), T()) for i in range(2)])
                Qnr = Ring([(sbt(ph, f"Qn{i}", [128, NOWN], BF16), T()) for i in range(2)])
                Qrr = Ring([(sbt(ph, f"Qr{i}", [128, NOWN], BF16), T()) for i in range(2)])
                for (Qr0, TQr0) in Qrr.items:
                    B.op(pool, lambda Qr0=Qr0: nc.gpsimd.memset(Qr0[64:128, :], 0.0), writes=[TQr0])
                PTr = Ring([(sbt(ph, f"PT{i}", [128, 512], BF16), T()) for i in range(4)])
                Sacr = Ring([(sbt(ph, f"Sac{i}", [128, 512], F32), T()) for i in range(2)])
                recr = Ring([(sbt(ph, f"rec{i}", [128, 512], F32), T()) for i in range(2)])
                Ost = Ring([(sbt(ph, f"Ost4{i}", [128, NOWN], BF16), T()) for i in range(2)])
                Vm_r = Vm.rearrange("(b p) n -> p b n", p=128)
                for (Os_, TOs_) in Ost.items:
                    B.op(pool, lambda Os_=Os_: nc.gpsimd.memset(Os_[:, NREAL:NOWN], 0.0), writes=[TOs_])

                def load_head4(h):
                    KTt, TKT = KTr.next()
                    B.dma(sp, KTt[:], KnT[h, :, 0:NTOK], writes=[TKT])
                    Vt, TV = Vr.next()
                    B.dma(sp, Vt[:], Vm_r[:, 0:65, h * 128:(h + 1) * 128], writes=[TV])
                    Qn, TQn = Qnr.next()
                    B.dma(sp, Qn[:], QnT[h], writes=[TQn])
                    Qr_, TQr = Qrr.next()
                    B.dma(sp, Qr_[0:64, :], QrT[h], writes=[TQr])
                    return KTt, TKT, Vt, TV, Qn, TQn, Qr_, TQr

                qgroups = [(0, 4, 512), (4, 4, 512), (8, 4, 512), (12, 4, 512), (16, 1, 32)]
                nxt = load_head4(0)
                for h in range(16):
                    KTt, TKT, Vt, TV, Qn, TQn, Qr_, TQr = nxt
                    if h + 1 < 16:
                        nxt = load_head4(h + 1)
                    Os, TOs = Ost.next()
                    for (m0, nq, ncols) in qgroups:
                        mend = m0 + nq
                        last = min(4 * (mend - 1) + 4, 64)
                        cbase = m0 * 128
                        po, Tpo = pacc.next()
                        Sac, TSac = Sacr.next()
                        def geom(kb):
                            m_lo = max(m0, -((4 - kb) // 4))
                            c_lo = (m_lo - m0) * 128
                            return m_lo, c_lo, ncols - c_lo, cbase + c_lo

                        pts = {}

                        def stA(kb):
                            m_lo, c_lo, N, q0 = geom(kb)
                            pz, Tpz = pf.next()
                            B.op(pe, lambda: nc.tensor.matmul(pz[:, 0:N], KTt[:, kb * 128:(kb + 1) * 128], Qn[:, q0:q0 + N],
                                                              start=True, stop=False), reads=[TKT, TQn], writes=[Tpz], sig=False)
                            B.op(pe, lambda: nc.tensor.matmul(pz[:, 0:N], KrTt[:, kb * 128:(kb + 1) * 128], Qr_[:, q0:q0 + N],
                                                              start=False, stop=True), reads=[TKr, TQr], writes=[Tpz])
                            PT, TPT = PTr.next()
                            B.op(act, lambda: nc.scalar.activation(out=PT[:, 0:N], in_=pz[:, 0:N], func=AF.Exp, scale=MLA_SCALE),
                                 reads=[Tpz], writes=[TPT])
                            for m in range(m_lo, mend):
                                if 4 * m <= kb <= 4 * m + 4:
                                    mi = kb - 4 * m
                                    cc = (m - m_lo) * 128
                                    w = min(128, N - cc)
                                    B.op(dve, lambda: nc.vector.tensor_tensor(PT[:, cc:cc + w], PT[:, cc:cc + w],
                                                                              mmb[:, mi * 128:mi * 128 + w], ALU.mult),
                                         reads=[TM], writes=[TPT])
                            if kb == 0:
                                B.op(dve, lambda: nc.vector.tensor_copy(Sac[:, 0:ncols], PT[:, 0:ncols]), reads=[TPT], writes=[TSac])
                            else:
                                B.op(dve, lambda: nc.vector.tensor_tensor(Sac[:, c_lo:ncols], Sac[:, c_lo:ncols], PT[:, 0:N], ALU.add),
                                     reads=[TPT], writes=[TSac])
                            pts[kb] = (PT, TPT)

                        def stB(kb):
                            m_lo, c_lo, N, q0 = geom(kb)
                            PT, TPT = pts.pop(kb)
                            B.op(pe, lambda: nc.tensor.matmul(po[:, c_lo:ncols], Vt[:, kb, :], PT[:, 0:N],
                                                              start=(kb == 0), stop=(kb == last)), reads=[TPT, TV], writes=[Tpo])

                        LAG4 = 2
                        for kb in range(last + 1 + LAG4):
                            if kb <= last:
                                stA(kb)
                            if kb - LAG4 >= 0:
                                stB(kb - LAG4)
                        ps, Tps = pf.next()
                        B.op(pe, lambda: nc.tensor.matmul(ps[:, 0:ncols], onesf[:, 0:128], Sac[:, 0:ncols], start=True, stop=True),
                             reads=[TSac, Tc], writes=[Tps])
                        rec, Trec = recr.next()
                        B.op(dve, lambda: nc.vector.reciprocal(rec[:, 0:ncols], ps[:, 0:ncols]), reads=[Tps], writes=[Trec])
                        B.op(dve, lambda: nc.vector.tensor_tensor(Os[:, cbase:cbase + ncols], po[:, 0:ncols], rec[:, 0:ncols], ALU.mult),
                             reads=[Tpo, Trec], writes=[TOs])
                    B.dma(sp, OT[8 + h], Os[:, :], reads=[TOs])
                B.barrier()

        if upto >= 5:
            with contextlib.ExitStack() as ph:
                Wpa = sbt(ph, "Wpa", [128, 8, D], BF16)
                Wpb = sbt(ph, "Wpb", [128, 16, D], BF16)
                TW = T()
                for c0 in range(0, 8, 4):
                    B.dma(pool, Wpa[:, c0:c0 + 4, :], w_proj_a.rearrange("(c p) n -> p c n", p=128)[:, c0:c0 + 4, :], writes=[TW])
                for c0 in range(0, 16, 4):
                    B.dma(pool, Wpb[:, c0:c0 + 4, :], w_proj_b.rearrange("(c p) n -> p c n", p=128)[:, c0:c0 + 4, :], writes=[TW])
                OTr = Ring([(sbt(ph, f"OTg{i}", [128, 24, 512], BF16), T()) for i in range(2)])
                gar = Ring([(sbt(ph, f"ga{i}", [128, 2, 512], BF16), T()) for i in range(3)])
                t1r = Ring([(sbt(ph, f"t1{i}", [128, 512], F32), T()) for i in range(2)])
                ysr = Ring([(sbt(ph, f"ys{i}", [128, 512], BF16), T()) for i in range(3)])
                OT_r = OT.rearrange("h p n -> p h n")
                for (c0, gw) in own_groups:
                    Og, TOg = OTr.next()
                    B.dma(sp, Og[:, 0:12, 0:gw], OT_r[:, 0:12, c0:c0 + gw], writes=[TOg])
                    B.dma(sp, Og[:, 12:24, 0:gw], OT_r[:, 12:24, c0:c0 + gw], writes=[TOg])
                    for f in range(16):
                        ga, Tga = gar.next()
                        B.dma(sp, ga[:, 0, 0:gw], GT[f, :, c0:c0 + gw], writes=[Tga])
                        B.dma(sp, ga[:, 1, 0:gw], GT[16 + f, :, c0:c0 + gw], writes=[Tga])
                        pa, Tpa = pf.next()
                        for hc in range(8):
                            B.op(pe, lambda hc=hc: nc.tensor.matmul(pa[:, 0:gw], Wpa[:, hc, f * 128:(f + 1) * 128], Og[:, hc, 0:gw],
                                                                    start=(hc == 0), stop=(hc == 7)), reads=[TW, TOg], writes=[Tpa], sig=(hc == 7))
                        pbk, Tpbk = pf.next()
                        for hc in range(16):
                            B.op(pe, lambda hc=hc: nc.tensor.matmul(pbk[:, 0:gw], Wpb[:, hc, f * 128:(f + 1) * 128], Og[:, 8 + hc, 0:gw],
                                                                    start=(hc == 0), stop=(hc == 15)), reads=[TW, TOg], writes=[Tpbk], sig=(hc == 15))
                        t1, Tt1 = t1r.next()
                        B.op(dve, lambda: nc.vector.tensor_tensor(t1[:, 0:gw], pa[:, 0:gw], ga[:, 0, 0:gw], ALU.mult), reads=[Tpa, Tga], writes=[Tt1])
                        ys, Tys = ysr.next()
                        B.op(dve, lambda: nc.vector.tensor_tensor(ys[:, 0:gw], pbk[:, 0:gw], ga[:, 1, 0:gw], ALU.mult), reads=[Tpbk, Tga], writes=[Tys])
                        B.op(pool, lambda: nc.gpsimd.tensor_tensor(ys[:, 0:gw], ys[:, 0:gw], t1[:, 0:gw], ALU.add), reads=[Tt1], writes=[Tys])
                        B.dma(sp, YT[f, :, c0:c0 + gw], ys[:, 0:gw], reads=[Tys])
                B.barrier()

        bc1 = nc.gpsimd.to_reg(NSLOT - 1)
        bc2 = nc.gpsimd.to_reg(2 * NSLOT - 1)
        SL = sbt(st, "SL", [128, 17, 2], I32)
        SLg = sbt(st, "SLg", [128, 17, 4], I32)
        WT = sbt(st, "WT", [128, 17, 2], F32)
        TSL = T()
        if upto >= 6:
            with contextlib.ExitStack() as ph:
                Wo = sbt(ph, "Wo", [128, 16, D], BF16)
                Wr = sbt(ph, "Wr", [128, 16, 36], F32)
                g2 = sbt(ph, "g2", [128, D], F32)
                brt = sbt(ph, "brt", [128, 36], F32)
                triu = sbt(ph, "triu", [128, 128], F32)
                eoff = sbt(ph, "eoff", [128, 32], F32)
                eoffi = sbt(ph, "eoffi", [128, 32], I32)
                carry = sbt(ph, "carry", [128, 32], F32)
                TW = T()
                Tcar = T()
                for c0 in range(0, 16, 4):
                    B.dma(pool, Wo[:, c0:c0 + 4, :], w_out.rearrange("(c p) n -> p c n", p=128)[:, c0:c0 + 4, :], writes=[TW])
                B.dma(sp, Wr[:, :, 0:4], w_rg.rearrange("(c p) n -> p c n", p=128), writes=[TW], allow_slow_non_contiguous=True)
                B.dma(sp, Wr[:, :, 4:36], w_re.rearrange("(c p) n -> p c n", p=128), writes=[TW], allow_slow_non_contiguous=True)
                B.dma(sp, g2[:], bc_rows(norm2_g, D), writes=[TW])
                B.dma(sp, brt[:, 0:4], bc_rows(b_rg, 4), writes=[TW])
                B.dma(sp, brt[:, 4:36], bc_rows(b_re, 32), writes=[TW])
                B.op(pool, lambda: nc.gpsimd.affine_select(triu[:], onesf[:, 0:128], [[1, 128]], ALU.is_ge, 0.0,
                                                           base=-1, channel_multiplier=-1), reads=[Tc], writes=[TW])
                B.op(pool, lambda: nc.gpsimd.iota(eoffi[:], [[CAP, 32]], base=0, channel_multiplier=0), writes=[TW])
                B.op(dve, lambda: nc.vector.tensor_copy(eoff[:], eoffi[:]), reads=[TW], writes=[TW])
                B.op(pool, lambda: nc.gpsimd.memset(carry[:], 0.0), writes=[Tcar])
                valid = sbt(ph, "valid", [128, 2], F32)
                B.op(pool, lambda: nc.gpsimd.affine_select(valid[:, 0:1], onesf[:, 0:1], [[0, 1]], ALU.is_ge, 0.0,
                                                           base=3, channel_multiplier=-1), reads=[Tc], writes=[TW])
                B.op(dve, lambda: nc.vector.tensor_scalar(valid[:, 1:2], valid[:, 0:1], -1.0e6, 1.0e6, ALU.mult, ALU.add),
                     reads=[TW], writes=[TW])

                yTr = Ring([(sbt(ph, f"yT{i}", [128, 16, 128], BF16), T()) for i in range(2)])
                xr = Ring([(sbt(ph, f"xo{i}", [128, D], F32), T()) for i in range(2)])
                h1r = Ring([(sbt(ph, f"h1{i}", [128, D], F32), T()) for i in range(2)])
                hn2r = Ring([(sbt(ph, f"hn2{i}", [128, D], F32), T()) for i in range(2)])
                hn2br = Ring([(sbt(ph, f"hn2b{i}", [128, D], BF16), T()) for i in range(2)])
                hn2Tr = Ring([(sbt(ph, f"hn2T{i}", [128, 16, 128], F32), T()) for i in range(2)])
                junk = sbt(ph, "junk6", [128, D], BF16)
                Tjunk = T()
                smr = Ring([(sbt(ph, f"sm6{i}", [128, 4], F32), T()) for i in range(3)])
                rtr = Ring([(sbt(ph, f"rt{i}", [128, 256], F32), T()) for i in range(2)])
                YT_r = YT.rearrange("f p n -> p f n")
                for t in range(17):
                    yT, TyT = yTr.next()
                    B.dma(sp, yT[:], YT_r[:, :, t * 128:(t + 1) * 128], writes=[TyT])
                    xt, Txt = xr.next()
                    B.dma(sp, xt[:], hown[t * 128:(t + 1) * 128, :], writes=[Txt])
                    h1, Th1 = h1r.next()
                    for bnk in range(4):
                        pz, Tpz = pf.next()
                        for fc in range(16):
                            B.op(pe, lambda fc=fc: nc.tensor.matmul(pz[:, :], yT[:, fc, :], Wo[:, fc, bnk * 512:(bnk + 1) * 512],
                                                                    start=(fc == 0), stop=(fc == 15)), reads=[TyT, TW], writes=[Tpz], sig=(fc == 15))
                        B.op(dve, lambda: nc.vector.tensor_tensor(h1[:, bnk * 512:(bnk + 1) * 512], pz[:, :], xt[:, bnk * 512:(bnk + 1) * 512], ALU.add),
                             reads=[Tpz, Txt], writes=[Th1])
                    B.dma(sp, H1[t * 128:(t + 1) * 128, :], h1[:], reads=[Th1])
                    sm, Tsm = smr.next()
                    rstd_from(h1[:], D, junk, sm, Th1, Tjunk, Tsm)
                    hn2, Thn2 = hn2r.next()
                    B.op(dve, lambda: nc.vector.scalar_tensor_tensor(out=hn2[:], in0=h1[:], scalar=sm[:, 2:3], in1=g2[:],
                                                                     op0=ALU.mult, op1=ALU.mult), reads=[Th1, Tsm, TW], writes=[Thn2])
                    hn2b, Thn2b = hn2br.next()
                    B.op(act, lambda: nc.scalar.copy(hn2b[:], hn2[:]), reads=[Thn2], writes=[Thn2b])
                    hn2T, Thn2T = hn2Tr.next()
                    for q in range(4):
                        pz, Tpz = pf.next()
                        for i in range(4):
                            cc = q * 4 + i
                            B.op(pe, lambda i=i, cc=cc: nc.tensor.transpose(pz[:, i * 128:(i + 1) * 128], hn2[:, cc * 128:(cc + 1) * 128], identf[:]),
                                 reads=[Thn2, Tc], writes=[Tpz], sig=(i == 3))
                        evac(hn2T[:, q * 4:(q + 1) * 4, :], pz[:, :].rearrange("p (a b) -> p a b", b=128), [Tpz], [Thn2T])
                    pl, Tpl = pf.next()
                    for cc in range(16):
                        B.op(pe, lambda cc=cc: nc.tensor.matmul(pl[:, 0:36], hn2T[:, cc, :], Wr[:, cc, :], start=(cc == 0), stop=(cc == 15)),
                             reads=[Thn2T, TW], writes=[Tpl], sig=(cc == 15))
                    rt, Trt = rtr.next()
                    L = rt[:, 0:36]
                    V = nc.vector
                    ops = []

                    def dv(fn, extra_r=()):
                        B.op(dve, fn, reads=[Trt] + list(extra_r), writes=[Trt])

                    dv(lambda: V.tensor_tensor(L, pl[:, 0:36], brt[:], ALU.add), [Tpl, TW])
                    gmax, ngmax, sumg = rt[:, 40:41], rt[:, 41:42], rt[:, 42:43]
                    ohg = rt[:, 44:48]
                    dv(lambda: V.reduce_max(gmax, rt[:, 0:4], axis=AX.X))
                    dv(lambda: V.tensor_scalar(ohg, rt[:, 0:4], gmax, None, ALU.is_equal))
                    dv(lambda: V.tensor_scalar(ngmax, gmax, -1.0, None, ALU.mult))
                    B.op(act, lambda: nc.scalar.activation(out=rt[:, 48:52], in_=rt[:, 0:4], func=AF.Exp, bias=ngmax, scale=1.0,
                                                           accum_out=sumg), reads=[Trt], writes=[Trt])
                    esel = rt[:, 56:64]
                    dv(lambda: V.tensor_scalar(esel, rt[:, 4:12], ohg[:, 0:1], None, ALU.mult))
                    for g_ in range(1, 4):
                        dv(lambda g_=g_: V.scalar_tensor_tensor(out=esel, in0=rt[:, 4 + 8 * g_:12 + 8 * g_], scalar=ohg[:, g_:g_ + 1],
                                                                in1=esel, op0=ALU.mult, op1=ALU.add))
                    top8 = rt[:, 64:72]
                    dv(lambda: V.max(top8, esel))
                    l1, l2 = rt[:, 64:65], rt[:, 65:66]
                    oh1, oh2 = rt[:, 72:80], rt[:, 80:88]
                    dv(lambda: V.tensor_scalar(oh1, esel, l1, None, ALU.is_equal))
                    dv(lambda: V.tensor_scalar(oh2, esel, l2, None, ALU.is_equal))
                    nl1, rr, den = rt[:, 88:89], rt[:, 89:90], rt[:, 90:91]
                    dv(lambda: V.tensor_scalar(nl1, l1, -1.0, None, ALU.mult))
                    B.op(act, lambda: nc.scalar.activation(out=rr, in_=l2, func=AF.Exp, bias=nl1, scale=1.0), reads=[Trt], writes=[Trt])
                    dv(lambda: V.tensor_scalar(den, rr, 1.0, sumg, ALU.add, ALU.mult))
                    dv(lambda: V.reciprocal(WT[:, t, 0:1], den), [TSL])
                    dv(lambda: V.tensor_tensor(WT[:, t, 1:2], WT[:, t, 0:1], rr, ALU.mult), [TSL])
                    E1, E2, Es = rt[:, 96:128], rt[:, 128:160], rt[:, 160:192]
                    for g_ in range(4):
                        dv(lambda g_=g_: V.tensor_scalar(rt[:, 96 + 8 * g_:104 + 8 * g_], oh1, ohg[:, g_:g_ + 1], None, ALU.mult))
                        dv(lambda g_=g_: V.tensor_scalar(rt[:, 128 + 8 * g_:136 + 8 * g_], oh2, ohg[:, g_:g_ + 1], None, ALU.mult))
                    dv(lambda: V.tensor_tensor(Es, E1, E2, ALU.add))
                    if t == 16:
                        dv(lambda: V.tensor_scalar(Es, Es, valid[:, 0:1], None, ALU.mult), [TW])
                    pp, Tpp = pf.next()
                    B.op(pe, lambda: nc.tensor.matmul(pp[:, 0:32], triu[:], Es, start=True, stop=True), reads=[Trt, TW], writes=[Tpp])
                    B.op(pe, lambda: nc.tensor.matmul(pp[:, 64:96], onesf[:, 0:128], Es, start=True, stop=True), reads=[Trt, Tc], writes=[Tpp])
                    posall, posoff, tmpm = rt[:, 192:224], rt[:, 224:256], rt[:, 8:40]
                    dv(lambda: V.tensor_tensor(posall, pp[:, 0:32], carry[:], ALU.add), [Tpp, Tcar])
                    B.op(dve, lambda: V.tensor_tensor(carry[:], pp[:, 64:96], carry[:], ALU.add), reads=[Tpp, Trt], writes=[Tcar])
                    dv(lambda: V.tensor_scalar(posoff, posall, float(CAP), 1.0e6, ALU.is_ge, ALU.mult))
                    dv(lambda: V.tensor_tensor(posoff, posoff, posall, ALU.add))
                    dv(lambda: V.tensor_tensor(posoff, posoff, eoff[:], ALU.add), [TW])
                    s1f, s2f = rt[:, 91:92], rt[:, 92:93]
                    dv(lambda: V.tensor_tensor(tmpm, E1, posoff, ALU.mult))
                    dv(lambda: V.reduce_sum(s1f, tmpm, axis=AX.X))
                    dv(lambda: V.tensor_tensor(tmpm, E2, posoff, ALU.mult))
                    dv(lambda: V.reduce_sum(s2f, tmpm, axis=AX.X))
                    if t == 16:
                        dv(lambda: V.tensor_scalar(s1f, s1f, valid[:, 1:2], None, ALU.add), [TW])
                        dv(lambda: V.tensor_scalar(s2f, s2f, valid[:, 1:2], None, ALU.add), [TW])
                    dv(lambda: V.tensor_copy(SL[:, t, 0:1], s1f), [TSL])
                    dv(lambda: V.tensor_copy(SL[:, t, 1:2], s2f), [TSL])
                    dv(lambda: V.tensor_scalar(SLg[:, t, 0:1], s1f, 2.0, None, ALU.mult), [TSL])
                    dv(lambda: V.tensor_scalar(SLg[:, t, 1:2], s1f, 2.0, 1.0, ALU.mult, ALU.add), [TSL])
                    dv(lambda: V.tensor_scalar(SLg[:, t, 2:3], s2f, 2.0, None, ALU.mult), [TSL])
                    dv(lambda: V.tensor_scalar(SLg[:, t, 3:4], s2f, 2.0, 1.0, ALU.mult, ALU.add), [TSL])
                    B.op(dve, lambda: V.tensor_copy(rt[:, 93:94], s2f), reads=[Trt], writes=[Trt, TSL])
                    for k in range(2):
                        B.idma(Xs, bass.IndirectOffsetOnAxis(SL[:, t, k:k + 1], 0), hn2b[:], None, reads=[TSL, Thn2b],
                               bounds_check=bc1, oob_is_err=False)
                B.barrier()

        if upto >= 7:
            with contextlib.ExitStack() as ph:
                Xr = Ring([(sbt(ph, f"Xe{i}", [128, D], BF16), T()) for i in range(2)])
                XTr = Ring([(sbt(ph, f"XT{i}", [128, 16, CAP], BF16), T()) for i in range(2)])
                W13r = Ring([(sbt(ph, f"W13{i}", [128, 16, 512], BF16), T()) for i in range(4)])
                W2r = Ring([(sbt(ph, f"W2{i}", [128, 8, 1024], BF16), T()) for i in range(2)])
                hidr = Ring([(sbt(ph, f"hid{i}", [128, 8, CAP], BF16), T()) for i in range(2)])
                s1r = Ring([(sbt(ph, f"s1{i}", [128, CAP], F32), T()) for i in range(2)])
                ystr = Ring([(sbt(ph, f"yst{i}", [128, 1024], F32), T()) for i in range(2)])
                ntile = CAP // 128
                for e in range(32):
                    XT, TXT = XTr.next()
                    for tt in range(ntile):
                        Xe, TXe = Xr.next()
                        r0 = e * CAP + tt * 128
                        B.dma(sp, Xe[:], Xs[r0:r0 + 128, :], writes=[TXe])
                        transposes(Xe, TXe, 16, 128, lambda i0, n, tt=tt: XT[:, i0:i0 + n, tt * 128:(tt + 1) * 128], TXT)
                    hid, Thid = hidr.next()
                    for half in range(2):
                        Wa, TWa = W13r.next()
                        B.dma(pool, Wa[:], w1[e].rearrange("(c p) n -> p c n", p=128)[:, :, half * 512:(half + 1) * 512], writes=[TWa])
                        Wb, TWb = W13r.next()
                        B.dma(pool, Wb[:], w3[e].rearrange("(c p) n -> p c n", p=128)[:, :, half * 512:(half + 1) * 512], writes=[TWb])
                        for fb in range(4):
                            p1, Tp1 = pf.next()
                            for c in range(16):
                                B.op(pe, lambda c=c: nc.tensor.matmul(p1[:, 0:CAP], Wa[:, c, fb * 128:(fb + 1) * 128], XT[:, c, :],
                                                                      start=(c == 0), stop=(c == 15)), reads=[TWa, TXT], writes=[Tp1], sig=(c == 15))
                            p3, Tp3 = pf.next()
                            for c in range(16):
                                B.op(pe, lambda c=c: nc.tensor.matmul(p3[:, 0:CAP], Wb[:, c, fb * 128:(fb + 1) * 128], XT[:, c, :],
                                                                      start=(c == 0), stop=(c == 15)), reads=[TWb, TXT], writes=[Tp3], sig=(c == 15))
                            s1, Ts1 = s1r.next()
                            B.op(act, lambda: nc.scalar.activation(out=s1[:], in_=p1[:, 0:CAP], func=AF.Silu), reads=[Tp1], writes=[Ts1])
                            B.op(dve, lambda: nc.vector.tensor_tensor(hid[:, half * 4 + fb, :], p3[:, 0:CAP], s1[:], ALU.mult),
                                 reads=[Tp3, Ts1], writes=[Thid])
                    for half in range(2):
                        W2t, TW2 = W2r.next()
                        B.dma(pool, W2t[:], w2[e].rearrange("(c p) n -> p c n", p=128)[:, :, half * 1024:(half + 1) * 1024], writes=[TW2])
                        for tt in range(ntile):
                            ys, Tys = ystr.next()
                            for bnk in range(2):
                                pz, Tpz = pf.next()
                                for fc in range(8):
                                    B.op(pe, lambda fc=fc: nc.tensor.matmul(pz[:, :], hid[:, fc, tt * 128:(tt + 1) * 128],
                                                                            W2t[:, fc, bnk * 512:(bnk + 1) * 512],
                                                                            start=(fc == 0), stop=(fc == 7)), reads=[Thid, TW2], writes=[Tpz], sig=(fc == 7))
                                evac(ys[:, bnk * 512:(bnk + 1) * 512], pz[:, :], [Tpz], [Tys])
                            r0 = e * CAP + tt * 128
                            B.dma(sp, Ys.rearrange("(s two) n -> s two n", two=2)[r0:r0 + 128, half, :], ys[:], reads=[Tys])
                B.barrier()

        out_toks = []
        if upto >= 8:
            with contextlib.ExitStack() as ph:
                gf = sbt(ph, "gf", [128, D], F32)
                TW = T()
                B.dma(sp, gf[:], bc_rows(final_g, D), writes=[TW])
                h1r = Ring([(sbt(ph, f"h1c{i}", [128, D], F32), T()) for i in range(2)])
                y1r = Ring([(sbt(ph, f"y1c{i}", [128, D], F32), T()) for i in range(2)])
                y2r = Ring([(sbt(ph, f"y2c{i}", [128, D], F32), T()) for i in range(2)])
                acr = Ring([(sbt(ph, f"acc{i}", [128, D], F32), T()) for i in range(2)])
                outr = Ring([(sbt(ph, f"oc{i}", [128, D], F32), T()) for i in range(2)])
                junk = sbt(ph, "junk8", [128, D], BF16)
                Tjunk = T()
                smr = Ring([(sbt(ph, f"sm8{i}", [128, 4], F32), T()) for i in range(3)])
                for t in range(17):
                    h1, Th1 = h1r.next()
                    B.dma(sp, h1[:], H1[t * 128:(t + 1) * 128, :], writes=[Th1])
                    ys = []
                    for k, ring in enumerate((y1r, y2r)):
                        y, Ty = ring.next()
                        B.op(pool, lambda y=y: nc.gpsimd.memset(y[:], 0.0), writes=[Ty])
                        for half in range(2):
                            B.idma(y[:, half * 1024:(half + 1) * 1024], None, Ys,
                                   bass.IndirectOffsetOnAxis(SLg[:, t, 2 * k + half:2 * k + half + 1], 0), reads=[TSL], writes=[Ty],
                                   bounds_check=bc2, oob_is_err=False)
                        ys.append((y, Ty))
                    ac, Tac = acr.next()
                    B.op(dve, lambda: nc.vector.scalar_tensor_tensor(out=ac[:], in0=ys[0][0][:], scalar=WT[:, t, 0:1], in1=h1[:],
                                                                     op0=ALU.mult, op1=ALU.add), reads=[ys[0][1], Th1, TSL], writes=[Tac])
                    B.op(dve, lambda: nc.vector.scalar_tensor_tensor(out=ac[:], in0=ys[1][0][:], scalar=WT[:, t, 1:2], in1=ac[:],
                                                                     op0=ALU.mult, op1=ALU.add), reads=[ys[1][1], TSL], writes=[Tac])
                    sm, Tsm = smr.next()
                    rstd_from(ac[:], D, junk, sm, Tac, Tjunk, Tsm)
                    oc, Toc = outr.next()
                    B.op(dve, lambda: nc.vector.scalar_tensor_tensor(out=oc[:], in0=ac[:], scalar=sm[:, 2:3], in1=gf[:],
                                                                     op0=ALU.mult, op1=ALU.mult), reads=[Tac, Tsm, TW], writes=[Toc])
                    out_toks.append(B.dma(sp, out[t * 128:(t + 1) * 128, :], oc[:], reads=[Toc]))
        B.barrier()
        for tk in out_toks:
            sp.wait(tk)
    return nc


def host_layout(inputs, c):
    b, j = c // 4, c % 4
    x = np.asarray(inputs["x"], dtype=np.float32)
    meta = np.asarray(inputs["meta_tokens"], dtype=np.float32)
    hall = np.zeros((NTP, D), np.float32)
    hall[0:16] = meta
    hall[16:16 + 8192] = x[b]
    hown = np.zeros((NOWN, D), np.float32)
    hown[0:NREAL] = hall[j:NTOK:4]
    half = 32
    inv = (10000.0 ** (-np.arange(half, dtype=np.float32) / half)).astype(np.float32)
    posk = np.arange(NTP, dtype=np.float32)
    angk = posk[:, None] * inv[None, :]
    ropek = np.concatenate([np.cos(angk), np.sin(angk)], axis=1).astype(np.float32)
    posq = (4 * np.arange(NOWN) + j).astype(np.float32)
    angq = posq[:, None] * inv[None, :]
    ropeq = np.concatenate([np.cos(angq), np.sin(angq)], axis=1).astype(np.float32)
    r = np.arange(128)
    kk = np.arange(512)
    sbmask = np.where(kk[None, :] < (4 * r[:, None] + j), 0.0, -30000.0).astype(np.float32)
    kk2 = np.arange(640)
    ck = np.floor_divide(kk2 - 16, 64)
    cq = np.floor_divide(4 * r + j - 16, 64)
    m2 = (ck[:, None] <= cq[None, :]).astype(np.float32)
    mlamask = m2.reshape(5, 128, 128).transpose(1, 0, 2).reshape(128, 640).copy()
    return dict(hall=hall, hown=hown, ropek=ropek, ropeq=ropeq, sbmask=sbmask, mlamask=mlamask)


_W2D = ["norm1_g", "b_gate", "kv_norm_g", "norm2_g", "b_route_group", "b_route_expert"]
_W3D = ["w_in", "w_uk", "w_uv", "w_proj_a", "w_proj_b", "w_out", "w_route_group", "w_route_expert"]


def make_in_maps(inputs):
    shared = {}
    for k in _W2D:
        shared[k] = np.ascontiguousarray(np.asarray(inputs[k], np.float32).reshape(1, -1))
    for k in _W3D:
        a = np.asarray(inputs[k], np.float32)
        shared[k] = np.ascontiguousarray(a.reshape(a.shape[-2], a.shape[-1]))
    for k in ("w1", "w3", "w2"):
        a = np.asarray(inputs[k], np.float32)
        shared[k] = np.ascontiguousarray(a.reshape(a.shape[-3], a.shape[-2], a.shape[-1]))
    shared["final_g"] = np.ascontiguousarray(np.asarray(inputs["final_g"], np.float32).reshape(1, -1))
    maps = []
    for c in range(8):
        m = dict(shared)
        m.update(host_layout(inputs, c))
        maps.append(m)
    return maps


def kernel(**inputs):
    nc = build_nc()
    in_maps = make_in_maps(inputs)
    res = run_bass_kernel_spmd(nc, in_maps, core_ids=list(range(8)))
    outp = np.zeros((2, 8192, D), np.float32)
    for c in range(8):
        b, j = c // 4, c % 4
        o = np.asarray(res.results[c]["out"])[0:NREAL]
        idx = 4 * np.arange(NREAL) + j
        sel = (idx >= 16) & (idx < 8208)
        outp[b, idx[sel] - 16] = o[sel]
    return outp
```
